# Optimizing a Trainium2 kernel written in Bass

```python
import math
import jax, jax.numpy as jnp
from jax import lax
import numpy as np

D_MODEL = 1024
BATCH = 32
SEQ = 2048
DEPTH = 2
DEC_BATCH = 2
DEC_SEQ = 8192
PAST_LEN = 128

MIX_WIDTH = D_MODEL
FOURIER_WIDTH = D_MODEL // 4
FOURIER_GROUPS = 4
FOURIER_GROUP_DIM = FOURIER_WIDTH // FOURIER_GROUPS
ATTN_WIDTH = MIX_WIDTH - FOURIER_WIDTH
DIFF_HEAD_DIM = 64
DIFF_V_DIM = 2 * DIFF_HEAD_DIM
N_DIFF_HEADS = ATTN_WIDTH // DIFF_V_DIM
QK_WIDTH = N_DIFF_HEADS * 2 * DIFF_HEAD_DIM
IN_PROJ_WIDTH = FOURIER_WIDTH + 2 * QK_WIDTH + ATTN_WIDTH
ROT_DIM = DIFF_HEAD_DIM // 4
ROPE_THETA = 500000.0
Q_BLOCK = 128
FF_DENSE = 2816
N_EXPERTS = 8
TOP_K = 2
FF_EXPERT = 3584
N_DENSE_LAYERS = (DEPTH + 1) // 2
N_MOE_LAYERS = DEPTH // 2
EPS = 1e-5

kernel_name = 'hybrid_fnet_diffattn_encoder'


def rmsnorm(x, g):
    xf = x.astype(jnp.float32)
    y = xf * lax.rsqrt(jnp.mean(xf * xf, axis=-1, keepdims=True) + EPS)
    return (y * g.astype(jnp.float32)).astype(x.dtype)


def swiglu(t, w_gate_up, w_down):
    gu = t @ w_gate_up
    g, u = jnp.split(gu, 2, axis=-1)
    return (jax.nn.silu(g) * u) @ w_down


def fourier_mix(u):
    B, S, _ = u.shape
    ug = u.astype(jnp.float32).reshape(B, S, FOURIER_GROUPS, FOURIER_GROUP_DIM)
    f = jnp.fft.fft2(ug, axes=(1, 3), norm='ortho')
    return jnp.real(f).reshape(B, S, FOURIER_WIDTH).astype(u.dtype)


def rope_partial(t, pos):
    inv = ROPE_THETA ** (-jnp.arange(0, ROT_DIM, 2, dtype=jnp.float32) / ROT_DIM)
    ang = pos.astype(jnp.float32)[:, None] * inv[None, :]
    cos = jnp.cos(ang)[None, :, None, None, :]
    sin = jnp.sin(ang)[None, :, None, None, :]
    tr = t[..., :ROT_DIM].astype(jnp.float32)
    x1, x2 = tr[..., :ROT_DIM // 2], tr[..., ROT_DIM // 2:]
    rot = jnp.concatenate([x1 * cos - x2 * sin, x2 * cos + x1 * sin], axis=-1).astype(t.dtype)
    return jnp.concatenate([rot, t[..., ROT_DIM:]], axis=-1)


def diff_attention(q, k, v, lam, subln_g, lam_init):
    B, S = q.shape[0], q.shape[1]
    nb = S // Q_BLOCK
    scale = DIFF_HEAD_DIM ** -0.5
    qb = q.reshape(B, nb, Q_BLOCK, N_DIFF_HEADS, 2, DIFF_HEAD_DIM).transpose(1, 0, 2, 3, 4, 5)

    def block(qi):
        s = jnp.einsum('bqhcd,bkhcd->bhcqk', qi, k,
                       preferred_element_type=jnp.float32) * scale
        p = jax.nn.softmax(s, axis=-1)
        w = p[:, :, 0] - lam * p[:, :, 1]
        return jnp.einsum('bhqk,bkhv->bqhv', w.astype(v.dtype), v)

    o = lax.map(block, qb)
    o = o.transpose(1, 0, 2, 3, 4).reshape(B, S, N_DIFF_HEADS, DIFF_V_DIM)
    o = rmsnorm(o, subln_g) * (1.0 - lam_init)
    return o.reshape(B, S, ATTN_WIDTH)


def moe_swiglu(h, router_w, e_gate_up, e_down):
    B, S, D = h.shape
    t = h.reshape(B * S, D)
    logits = (t @ router_w).astype(jnp.float32)
    vals, idx = lax.top_k(logits, TOP_K)
    gates = jax.nn.softmax(vals, axis=-1)
    comb = jnp.sum(jax.nn.one_hot(idx, N_EXPERTS, dtype=jnp.float32) * gates[..., None], axis=1)
    out = jnp.zeros_like(t)
    for e in range(N_EXPERTS):
        out = out + comb[:, e:e + 1].astype(t.dtype) * swiglu(t, e_gate_up[e], e_down[e])
    return out.reshape(B, S, D)


def trunk(x, norm_mix, w_in, lambda_qk, subln_gain, w_out, norm_ffn,
          ffn_w_gate_up, ffn_w_down, router_w, expert_w_gate_up, expert_w_down, final_norm):
    B, S, _ = x.shape
    pos = jnp.arange(S, dtype=jnp.int32)
    for i in range(DEPTH):
        lam_init = 0.8 - 0.6 * math.exp(-0.3 * i)
        h = rmsnorm(x, norm_mix[i])
        proj = h @ w_in[i]
        u_f, q, k, v = jnp.split(
            proj, [FOURIER_WIDTH, FOURIER_WIDTH + QK_WIDTH, FOURIER_WIDTH + 2 * QK_WIDTH], axis=-1)
        q = rope_partial(q.reshape(B, S, N_DIFF_HEADS, 2, DIFF_HEAD_DIM), pos)
        k = rope_partial(k.reshape(B, S, N_DIFF_HEADS, 2, DIFF_HEAD_DIM), pos)
        v = v.reshape(B, S, N_DIFF_HEADS, DIFF_V_DIM)
        lq = lambda_qk[i].astype(jnp.float32)
        lam = jnp.exp(jnp.sum(lq[0] * lq[1])) - jnp.exp(jnp.sum(lq[2] * lq[3])) + lam_init
        o_attn = diff_attention(q, k, v, lam, subln_gain[i], lam_init)
        o_f = fourier_mix(u_f)
        x = x + jnp.concatenate([o_f, o_attn], axis=-1) @ w_out[i]
        h = rmsnorm(x, norm_ffn[i])
        if i % 2 == 0:
            x = x + swiglu(h, ffn_w_gate_up[i // 2], ffn_w_down[i // 2])
        else:
            j = i // 2
            x = x + moe_swiglu(h, router_w[j], expert_w_gate_up[j], expert_w_down[j])
    return rmsnorm(x, final_norm)


def setup_inputs(seed: int = 0) -> dict:
    key = jax.random.key(seed)
    ks = jax.random.split(key, 16)
    f32 = jnp.float32
    nrm = lambda k, shp, s: jax.random.normal(k, shp, f32) * s
    return {
        'x_prompt': nrm(ks[0], (BATCH, SEQ, D_MODEL), 1.0),
        'x_sample': nrm(ks[1], (DEC_BATCH, DEC_SEQ, D_MODEL), 1.0),
        'norm_mix': 1.0 + nrm(ks[2], (DEPTH, D_MODEL), 0.01),
        'w_in': nrm(ks[3], (DEPTH, D_MODEL, IN_PROJ_WIDTH), D_MODEL ** -0.5),
        'lambda_qk': nrm(ks[4], (DEPTH, 4, DIFF_HEAD_DIM), 0.1),
        'subln_gain': 1.0 + nrm(ks[5], (DEPTH, DIFF_V_DIM), 0.01),
        'w_out': nrm(ks[6], (DEPTH, MIX_WIDTH, D_MODEL), MIX_WIDTH ** -0.5),
        'norm_ffn': 1.0 + nrm(ks[7], (DEPTH, D_MODEL), 0.01),
        'ffn_w_gate_up': nrm(ks[8], (N_DENSE_LAYERS, D_MODEL, 2 * FF_DENSE), D_MODEL ** -0.5),
        'ffn_w_down': nrm(ks[9], (N_DENSE_LAYERS, FF_DENSE, D_MODEL), FF_DENSE ** -0.5),
        'router_w': nrm(ks[10], (N_MOE_LAYERS, D_MODEL, N_EXPERTS), D_MODEL ** -0.5),
        'expert_w_gate_up': nrm(ks[11], (N_MOE_LAYERS, N_EXPERTS, D_MODEL, 2 * FF_EXPERT), D_MODEL ** -0.5),
        'expert_w_down': nrm(ks[12], (N_MOE_LAYERS, N_EXPERTS, FF_EXPERT, D_MODEL), FF_EXPERT ** -0.5),
        'final_norm': 1.0 + nrm(ks[13], (D_MODEL,), 0.01),
    }


def reference(x_prompt, x_sample, norm_mix, w_in, lambda_qk, subln_gain, w_out, norm_ffn,
              ffn_w_gate_up, ffn_w_down, router_w, expert_w_gate_up, expert_w_down, final_norm):
    y_prompt = trunk(x_prompt, norm_mix, w_in, lambda_qk, subln_gain, w_out, norm_ffn,
                     ffn_w_gate_up, ffn_w_down, router_w, expert_w_gate_up, expert_w_down, final_norm)
    y_sample = trunk(x_sample, norm_mix, w_in, lambda_qk, subln_gain, w_out, norm_ffn,
                     ffn_w_gate_up, ffn_w_down, router_w, expert_w_gate_up, expert_w_down, final_norm)
    return (y_prompt, y_sample)
```

```python
import math
from contextlib import ExitStack

import numpy as np
import ml_dtypes
import concourse.bass as bass
import concourse.mybir as mybir
from concourse.bass_utils import run_bass_kernel_spmd

F32 = mybir.dt.float32
BF16 = mybir.dt.bfloat16
AF = mybir.ActivationFunctionType
ALU = mybir.AluOpType
AX = mybir.AxisListType

P = 128
D = 1024
KD = 8
SEG = 2048
NT = 4
TW = 512
NH = 6
FF_D = 2816
FF_E = 3584
NE = 8
EPS = 1e-5
LAM_INIT = [0.8 - 0.6 * math.exp(-0.3 * i) for i in range(2)]
NRING = 5
SLAB = 4096


class Op:
    __slots__ = ("eng", "fn", "deps", "idx", "marked", "sem", "semval", "dma")


class Prog:
    ENGS = ("pe", "act", "dve", "pool", "sp")

    def __init__(self):
        self.ops = []
        self.last_w = {}
        self.readers = {}
        self.last_on_eng = {}
        self.pending_barrier = {}
        self.dmas_since_barrier = []
        self.dma_count = {"pool": 0, "sp": 0}
        self.dma_sem_last = {}

    def op(self, eng, fn, reads=(), writes=(), dma=False):
        o = Op()
        o.eng, o.fn, o.dma, o.idx = eng, fn, dma, len(self.ops)
        o.marked = dma
        writes = list(writes) + [k for k in reads if k[0] == "ps" and k not in writes]
        deps = set()
        for k in reads:
            w = self.last_w.get(k)
            if w is not None:
                deps.add(w)
        for k in writes:
            w = self.last_w.get(k)
            if w is not None:
                deps.add(w)
            r = self.readers.get(k)
            if r:
                deps.update(r.values())
        pb = self.pending_barrier.pop(eng, None)
        if pb:
            deps.update(pb)
        for k in reads:
            self.readers.setdefault(k, {})[("d", o.idx) if dma else eng] = o.idx
        for k in writes:
            self.last_w[k] = o.idx
            self.readers[k] = {}
        if dma:
            n = self.dma_count[eng]
            self.dma_count[eng] = n + 1
            slot = (eng, n % NDS)
            prev = self.dma_sem_last.get(slot)
            if prev is not None:
                deps.add(prev)
            self.dma_sem_last[slot] = o.idx
            o.sem = slot
            o.semval = 16 * (n // NDS + 1)
            self.dmas_since_barrier.append(o.idx)
        deps.discard(o.idx)
        o.deps = deps
        self.ops.append(o)
        self.last_on_eng[eng] = o.idx
        return o

    def barrier(self):
        s = set(self.last_on_eng.values())
        s.update(self.dmas_since_barrier)
        self.dmas_since_barrier = []
        for e in self.ENGS:
            self.pending_barrier.setdefault(e, set()).update(s)

    def finalize(self):
        ops = self.ops
        for o in ops:
            keep = set()
            for d in o.deps:
                p = ops[d]
                if (not p.dma) and (not o.dma) and p.eng == o.eng and o.eng == "pe":
                    continue
                keep.add(d)
                p.marked = True
            o.deps = keep
        cnt = {e: 0 for e in self.ENGS}
        for o in ops:
            if o.dma:
                continue
            if o.marked:
                cnt[o.eng] += 1
                c = cnt[o.eng]
                o.sem = (o.eng, "c", (c - 1) // EPOCH)
                o.semval = (c - 1) % EPOCH + 1
        self.n_epochs = {e: max(cnt[e] - 1, 0) // EPOCH + 1 for e in self.ENGS}


NDS = 24
EPOCH = 20000


def build_program(n_prompt=4, do_sample=True, debug=False):
    nc = bass.Bass("TRN2", target_bir_lowering=False)
    pg = Prog()

    def din(name, shape, dt=F32):
        return nc.dram_tensor(name, list(shape), dt, kind="ExternalInput").ap()

    def dout(name, shape, dt=F32):
        return nc.dram_tensor(name, list(shape), dt, kind="ExternalOutput").ap()

    def dscr(name, shape, dt=BF16):
        return nc.dram_tensor(name, list(shape), dt).ap()

    NP_ = max(n_prompt, 1)
    xp = din("xp", [NP_, D, SEG])
    yp = dout("yp", [NP_, D, SEG])
    xs = din("xs", [4, D, SEG])
    ys = dout("ys", [D, SEG])
    w_in = din("w_in", [2, D, 2560])
    w_out = din("w_out", [2, D, D])
    w_gu = din("w_gu", [D, 2 * FF_D])
    w_d = din("w_d", [FF_D, D])
    w_r = din("w_r", [P, KD * NE])
    e_gu = din("e_gu", [NE, D, 2 * FF_E])
    e_d = din("e_d", [NE, FF_E, D])
    gn_d = din("gn", [P, 40])
    sub_d = din("sub", [P, 2])
    lqk_d = din("lqk", [P, 512])
    ropeP = din("ropeP", [2, P, SEG])
    ropeS = din("ropeS", [4, 2, P, SEG])
    dftP = din("dftP", [4, 1, 4, P, SLAB], BF16)
    dftS = din("dftS", [4, 4, 4, 4, P, SLAB], BF16)
    cst_d = din("cst", [P, 512], BF16)
    ident_d = din("ident", [P, P])
    sel_d = din("sel", [NE, NE * P], BF16)

    if debug:
        dbg_mix = [dout(f"dbg_mix{l}", [D, SEG], BF16) for l in range(2)]
        dbg_h = [dout(f"dbg_h{l}", [D, SEG], BF16) for l in range(2)]
        dbg_q = [dout(f"dbg_q{l}", [768, SEG], BF16) for l in range(2)]
        dbg_xm = [dout(f"dbg_xm{l}", [D, SEG]) for l in range(2)]
        dbg_xf = [dout(f"dbg_xf{l}", [D, SEG]) for l in range(2)]
        dbg_k = [dout(f"dbg_k{l}", [768, SEG], BF16) for l in range(2)]
        dbg_v = [dout(f"dbg_v{l}", [768, SEG], BF16) for l in range(2)]
        dbg_g = [dout(f"dbg_g{l}", [P, 8192], BF16) for l in range(2)]
    kP = dscr("kP", [1, 768, SEG]); vP = dscr("vP", [1, 768, SEG]); gP = dscr("gP", [1, P, 8192])
    kS = [dscr(f"kS{l}", [4, 768, SEG]) for l in range(2)]
    vS = [dscr(f"vS{l}", [4, 768, SEG]) for l in range(2)]
    gS = [dscr(f"gS{l}", [4, P, 8192]) for l in range(2)]

    es = ExitStack()
    with es:
        def sb(name, shape, dt):
            return es.enter_context(nc.sbuf_tensor("sb_" + name, list(shape), dt))

        xT = sb("xT", [P, KD * SEG], F32)
        mix = sb("mix", [P, KD * SEG], BF16)
        R = sb("R", [P, 20480], BF16)
        ring = sb("ring", [P, NRING * SLAB], BF16)
        cst = sb("cst", [P, 512], BF16)
        ident = sb("ident", [P, P], F32)
        sel = sb("sel", [NE, NE * P], BF16)
        gn = sb("gn", [P, 40], F32)
        subg = sb("subg", [P, 2], F32)
        lqk = sb("lqk", [P, 512], F32)
        sm = sb("sm", [P, 64], F32)
        misc = sb("misc", [P, 32], F32)
        gwr = sb("gwr", [P, KD * NE], F32)
        wr_s = sb("wr_s", [P, KD * NE], F32)
        onesf = sb("onesf", [P, P], F32)
        T = sb("T", [P, 12288], BF16)
        ps = [es.enter_context(nc.psum_tensor(f"ps{i}", [P, TW], F32)) for i in range(8)]

        ones = cst[:, 0:128]
        Pm = cst[:, 128:256]
        BD = cst[:, 256:512]

        def x_(kc, t, w=TW, off=0):
            return xT[:, kc * SEG + t * TW + off: kc * SEG + t * TW + off + w]

        def mx(kc, t, w=TW, off=0):
            return mix[:, kc * SEG + t * TW + off: kc * SEG + t * TW + off + w]

        def q_(h, t, lo=0, hi=P):
            return R[lo:hi, h * SEG + t * TW: h * SEG + (t + 1) * TW]

        def kv_slot(s):
            b = 12288 + s * 4096
            return R[:, b:b + 2048], R[:, b + 2048:b + 4096]

        rope_cos = R[:, 12288:16384].bitcast(F32)
        rope_sin = R[:, 16384:20480].bitcast(F32)
        Gslot = [R[:, 0:8192], R[:, 12288:20480]]

        def aT(par, ci, t):
            b = par * 8192 + ci * SEG + t * TW
            return R[:, b:b + TW]

        def bc_(par, t):
            b = 16384 + par * SEG + t * TW
            return R[:, b:b + TW]

        def rslot(i):
            return ring[:, i * SLAB:(i + 1) * SLAB]

        def tb(off, n):
            return T[:, off:off + n]

        def tf(off, n):
            return T[:, off:off + n].bitcast(F32)

        sq = [tb(0, 512), tb(512, 512)]
        rstd = [tf(1024, 1024), tf(2048, 1024)]
        qb = [tb(3072, 512), tb(3584, 512)]
        tt = tf(4096, 1024)
        uu = tf(5120, 1024)
        vst = [tb(6144, 768), tb(6912, 768)]
        gst = tb(7680, 2048)
        kst = [tb(9728, 512), tb(10240, 512)]
        uTt = [tb(10752, 512), tb(11264, 512)]
        accb = [[tf(0, 1024), tf(1024, 1024)], [tf(2048, 1024), tf(3072, 1024)]]
        E2b = [[tb(4096 + (c * 3 + k) * 512, 512) for k in range(3)] for c in range(2)]
        rr = tf(7168, 1024)
        t1 = tf(8192, 1024)
        t2 = tf(9216, 1024)
        oo = tf(10240, 1024)
        osq = tb(11264, 512)
        sg = [tf(3072, 1024), tf(4096, 1024)]
        tmpf = [tf(5120, 1024), tf(6144, 1024)]
        combT = T[0:NE, 7168:9216]

        ring_ctr = [0]

        def pe_mm(out, lhsT, rhs, start, stop, reads, writes):
            return pg.op("pe", lambda e, out=out, lhsT=lhsT, rhs=rhs, start=start, stop=stop:
                         e.matmul(out, lhsT, rhs, start=start, stop=stop), reads, writes)

        def act(out, in_, func, reads, writes, scale=None, bias=None):
            def f(e, out=out, in_=in_, func=func, scale=scale, bias=bias):
                kw = {}
                if scale is not None:
                    kw["scale"] = scale
                if bias is not None:
                    kw["bias"] = bias
                return e.activation(out=out, in_=in_, func=func, **kw)
            return pg.op("act", f, reads, writes)

        def dve(fn, reads, writes):
            return pg.op("dve", fn, reads, writes)

        def dma(eng, out, in_, reads, writes):
            return pg.op(eng, lambda e, out=out, in_=in_: e.dma_start(out=out, in_=in_), reads, writes, dma=True)

        def load_slab_w(W2d, r0, nk, c0, ncols):
            i = ring_ctr[0] % NRING
            ring_ctr[0] += 1
            sl = rslot(i)
            dma("pool", sl[:, 0:nk * ncols].rearrange("p (k c) -> p k c", k=nk),
                W2d[r0:r0 + P * nk, c0:c0 + ncols].rearrange("(k p) c -> p k c", p=P),
                [], [("ring", i)])
            return i

        def load_slab_t(src2d):
            i = ring_ctr[0] % NRING
            ring_ctr[0] += 1
            dma("sp", rslot(i), src2d, [], [("ring", i)])
            return i

        def norm_tile(t, gcol, moe=False):
            for kc in range(KD):
                s_ = sq[kc % 2]
                act(s_, x_(kc, t), AF.Square, [("x", kc, t)], [("sq", kc % 2)])
                pe_mm(ps[7][:], ones, s_, kc == 0, kc == KD - 1, [("sq", kc % 2)], [("ps", 7)])
            r_ = rstd[t % 2]
            act(r_, ps[7][:], AF.Sqrt, [("ps", 7)], [("rstd", t % 2)], scale=1.0 / D, bias=EPS)
            dve(lambda e, r_=r_: e.reciprocal(r_, r_), [("rstd", t % 2)], [("rstd", t % 2)])
            if moe:
                route_tile(t, r_)
            for kc in range(KD):
                dve(lambda e, kc=kc, t=t, r_=r_: e.scalar_tensor_tensor(
                    out=mx(kc, t), in0=x_(kc, t), scalar=gn[:, gcol + kc:gcol + kc + 1], in1=r_,
                    op0=ALU.mult, op1=ALU.mult),
                    [("x", kc, t), ("rstd", t % 2)], [("mix", kc, t)])

        def route_tile(t, r_):
            for sub in range(4):
                j = 4 * t + sub
                pg.op("pe", lambda e, sub=sub, r_=r_: e.transpose(ps[5][:, 0:P], r_[:, sub * P:(sub + 1) * P], ident[:]),
                      [("rstd", t % 2)], [("ps", 5)])
                for kc in range(KD):
                    pe_mm(ps[6][:, 0:NE], x_(kc, t, P, sub * P), gwr[:, kc * NE:(kc + 1) * NE], kc == 0, kc == KD - 1,
                          [("x", kc, t), ("gwr",)], [("ps", 6)])
                K = ("sm",)
                dve(lambda e: e.tensor_copy(sm[:, 0:1], ps[5][:, 0:1]), [("ps", 5)], [K])
                dve(lambda e: e.tensor_copy(sm[:, 8:16], ps[6][:, 0:NE]), [("ps", 6)], [K])
                dve(lambda e: e.reduce_max(sm[:, 16:17], sm[:, 8:16], axis=AX.X), [K], [K])
                dve(lambda e: e.tensor_scalar(out=sm[:, 24:32], in0=sm[:, 8:16], scalar1=sm[:, 16:17], scalar2=None,
                                              op0=ALU.is_equal), [K], [K])
                dve(lambda e: e.scalar_tensor_tensor(out=sm[:, 32:40], in0=sm[:, 24:32], scalar=-1e30, in1=sm[:, 8:16],
                                                     op0=ALU.mult, op1=ALU.add), [K], [K])
                dve(lambda e: e.reduce_max(sm[:, 17:18], sm[:, 32:40], axis=AX.X), [K], [K])
                dve(lambda e: e.tensor_scalar(out=sm[:, 40:48], in0=sm[:, 32:40], scalar1=sm[:, 17:18], scalar2=None,
                                              op0=ALU.is_equal), [K], [K])
                dve(lambda e: e.tensor_tensor(out=sm[:, 18:19], in0=sm[:, 16:17], in1=sm[:, 17:18], op=ALU.subtract),
                    [K], [K])
                act(sm[:, 19:20], sm[:, 18:19], AF.Sigmoid, [K], [K], scale=sm[:, 0:1])
                dve(lambda e: e.tensor_scalar(out=sm[:, 20:21], in0=sm[:, 19:20], scalar1=-1.0, scalar2=1.0,
                                              op0=ALU.mult, op1=ALU.add), [K], [K])
                dve(lambda e: e.tensor_scalar(out=sm[:, 48:56], in0=sm[:, 24:32], scalar1=sm[:, 19:20], scalar2=None,
                                              op0=ALU.mult), [K], [K])
                dve(lambda e: e.scalar_tensor_tensor(out=sm[:, 56:64], in0=sm[:, 40:48], scalar=sm[:, 20:21],
                                                     in1=sm[:, 48:56], op0=ALU.mult, op1=ALU.add), [K], [K])
                pg.op("pe", lambda e: e.transpose(ps[5][0:NE, P:2 * P], sm[:, 56:64], ident[:]), [K], [("ps", 5)])
                act(combT[:, j * P:(j + 1) * P], ps[5][0:NE, P:2 * P], AF.Copy, [("ps", 5)], [("combT", j)])

        def inproj(layer, rope_src, scr, blk, do_q=True, do_kvg=True):
            kscr, vscr, gscr = scr
            pg.barrier()
            for t in range(NT):
                norm_tile(t, layer * 8)
            dma("sp", rope_cos, rope_src[0], [], [("ropec",)])
            dma("sp", rope_sin, rope_src[1], [], [("ropes",)])
            W = w_in[layer]
            rot = [0]
            prot = [0]

            def proj_chunk(slot, ci, t):
                bank = rot[0] % 3
                rot[0] += 1
                for kc in range(KD):
                    pe_mm(ps[bank][:], rslot(slot)[:, kc * 512 + ci * P: kc * 512 + (ci + 1) * P], mx(kc, t),
                          kc == 0, kc == KD - 1, [("ring", slot), ("mix", kc, t)], [("ps", bank)])
                return bank

            def rope_evac(bank, t, out_ap, out_key):
                pb = 3 + prot[0] % 2
                qq = qb[prot[0] % 2]
                qk = ("qb", prot[0] % 2)
                prot[0] += 1
                act(qq, ps[bank][:], AF.Copy, [("ps", bank)], [qk])
                pe_mm(ps[pb][:], Pm, qq, True, True, [qk], [("ps", pb)])
                dve(lambda e, bank=bank, t=t: e.tensor_tensor(out=tt, in0=ps[bank][:], in1=rope_cos[:, t * TW:(t + 1) * TW],
                                                              op=ALU.mult), [("ps", bank), ("ropec",)], [("tt",)])
                dve(lambda e, pb=pb, t=t: e.tensor_tensor(out=uu, in0=ps[pb][:], in1=rope_sin[:, t * TW:(t + 1) * TW],
                                                          op=ALU.mult), [("ps", pb), ("ropes",)], [("uu",)])
                dve(lambda e, out_ap=out_ap: e.tensor_tensor(out=out_ap, in0=tt, in1=uu, op=ALU.add),
                    [("tt",), ("uu",)], [out_key])

            slots = {}
            slots[0] = load_slab_w(W, 0, KD, 0, 512)
            if do_kvg:
                for t in range(NT):
                    for cc in range(2):
                        bank = proj_chunk(slots[0], cc, t)
                        ut = uTt[cc]
                        act(ut, ps[bank][:], AF.Copy, [("ps", bank)], [("uTt", cc)])
                        for sub in range(4):
                            tbk = 5 + (sub % 2)
                            pe_mm(ps[tbk][:, 0:256], ut[:, sub * P:(sub + 1) * P], BD, True, True,
                                  [("uTt", cc)], [("ps", tbk)])
                            act(gst[:, sub * 512 + cc * 256: sub * 512 + (cc + 1) * 256], ps[tbk][:, 0:256], AF.Copy,
                                [("ps", tbk)], [("gst",)])
                    dma("sp", gscr[blk][:, t * 2048:(t + 1) * 2048], gst, [("gst",)], [("gscr", blk)])
            if do_q:
                for ci in (2, 3):
                    h = ci - 2
                    for t in range(NT):
                        bank = proj_chunk(slots[0], ci, t)
                        rope_evac(bank, t, q_(h, t), ("q", h, t))
            if do_q:
                slots[1] = load_slab_w(W, 0, KD, 512, 512)
                for ci in range(4):
                    h = 2 + ci
                    for t in range(NT):
                        bank = proj_chunk(slots[1], ci, t)
                        rope_evac(bank, t, q_(h, t), ("q", h, t))
            if do_kvg:
                kctr = [0]

                def k_chunk(slot, ci, h):
                    for t in range(NT):
                        bank = proj_chunk(slot, ci, t)
                        ks = kst[kctr[0] % 2]
                        kk = ("kst", kctr[0] % 2)
                        kctr[0] += 1
                        rope_evac(bank, t, ks, kk)
                        dma("sp", kscr[blk][h * P:(h + 1) * P, t * TW:(t + 1) * TW], ks, [kk], [("kscr", blk)])
                slots[2] = load_slab_w(W, 0, KD, 1024, 512)
                for ci in range(4):
                    k_chunk(slots[2], ci, ci)
                slots[3] = load_slab_w(W, 0, KD, 1536, 512)
                for ci in range(2):
                    k_chunk(slots[3], ci, 4 + ci)
                slots[4] = load_slab_w(W, 0, KD, 2048, 512)
                vview = vscr[blk].rearrange("(h t) (j v) -> t h j v", t=P, v=P)
                for j in range(16):
                    t, sub = j // 4, j % 4
                    vs_ = vst[j % 2]
                    vk = ("vst", j % 2)
                    for kc in range(KD):
                        pe_mm(ps[5][:, 0:256], mx(kc, t, P, sub * P), rslot(slots[3])[:, kc * 512 + 256: kc * 512 + 512],
                              kc == 0, kc == KD - 1, [("ring", slots[3]), ("mix", kc, t)], [("ps", 5)])
                    act(vs_[:, 0:256], ps[5][:, 0:256], AF.Copy, [("ps", 5)], [vk])
                    for kc in range(KD):
                        pe_mm(ps[6][:], mx(kc, t, P, sub * P), rslot(slots[4])[:, kc * 512: kc * 512 + 512],
                              kc == 0, kc == KD - 1, [("ring", slots[4]), ("mix", kc, t)], [("ps", 6)])
                    act(vs_[:, 256:768], ps[6][:], AF.Copy, [("ps", 6)], [vk])
                    dma("sp", vview[:, :, j, :], vs_.rearrange("t (h v) -> t h v", v=P), [vk], [("vscr", blk)])

        def attention(layer, scr, nblk):
            kscr, vscr, gscr = scr
            pg.barrier()
            units = [(h, t) for h in range(NH) for t in range(NT)]
            steps = [(ui, b, j) for ui in range(len(units)) for b in range(nblk) for j in range(16)]
            buse = []
            step_bu = []
            for (ui, b, j) in steps:
                h = units[ui][0]
                if not buse or buse[-1] != (h, b):
                    buse.append((h, b))
                step_bu.append(len(buse) - 1)
            last_step_of_bu = {}
            for i, n_ in enumerate(step_bu):
                last_step_of_bu[n_] = i

            def load_bu(n_):
                if n_ >= len(buse):
                    return
                h, b = buse[n_]
                s_ = n_ % 2
                Kt, Vt = kv_slot(s_)
                dma("sp", Kt, kscr[b][h * P:(h + 1) * P, :], [("kscr", b)], [("kv", s_)])
                dma("sp", Vt, vscr[b][h * P:(h + 1) * P, :], [("vscr", b)], [("kv", s_)])

            load_bu(0)
            load_bu(1)
            deferred = []

            def emit_S(i):
                ui, b, j = steps[i]
                h, t = units[ui]
                par = ui % 2
                s_ = step_bu[i] % 2
                Kt, Vt = kv_slot(s_)
                k2 = i % 2
                first = (b == 0 and j == 0)
                for c in range(2):
                    bank = 2 * k2 + c
                    pe_mm(ps[bank][:], Kt[c * 64:(c + 1) * 64, j * P:(j + 1) * P], q_(h, t, c * 64, (c + 1) * 64),
                          True, True, [("kv", s_), ("q", h, t)], [("ps", bank)])
                for c in range(2):
                    bank = 2 * k2 + c
                    ei = i % 3
                    act(E2b[c][ei], ps[bank][:], AF.Exp, [("ps", bank)], [("E", c, ei)], scale=0.125)

            def unit_end(ui, i):
                h, t = units[ui]
                for c in range(2):
                    dve(lambda e, c=c: e.reciprocal(rr, ps[6 + c][:]), [("ps", 6 + c)], [("rr",)])
                    tc_ = t1 if c == 0 else t2
                    tk = ("t1",) if c == 0 else ("t2",)
                    dve(lambda e, c=c, tc_=tc_: e.tensor_tensor(out=tc_, in0=ps[4 + c][:], in1=rr, op=ALU.mult),
                        [("ps", 4 + c), ("rr",)], [tk])
                dve(lambda e, layer=layer, h=h, t=t: e.scalar_tensor_tensor(
                    out=mx(2 + h, t), in0=t2, scalar=misc[:, 4 + layer:5 + layer], in1=t1, op0=ALU.mult, op1=ALU.add),
                    [("t1",), ("t2",), ("misc",)], [("mix", 2 + h, t)])

            def emit_PV(i):
                ui, b, j = steps[i]
                s_ = step_bu[i] % 2
                _, Vt = kv_slot(s_)
                first = (b == 0 and j == 0)
                last = (b == nblk - 1 and j == 15)
                ei = i % 3
                for c in range(2):
                    pe_mm(ps[4 + c][:], Vt[:, j * P:(j + 1) * P], E2b[c][ei], first, last,
                          [("kv", s_), ("E", c, ei)], [("ps", 4 + c)])
                for c in range(2):
                    pe_mm(ps[6 + c][:], ones, E2b[c][ei], first, last, [("E", c, ei)], [("ps", 6 + c)])
                if last:
                    unit_end(ui, i)

            n = len(steps)
            emit_S(0)
            for i in range(n):
                if i + 1 < n:
                    emit_S(i + 1)
                emit_PV(i)
                for item in list(deferred):
                    if item[0] <= i:
                        item[1]()
                        deferred.remove(item)
                bu = step_bu[i]
                if last_step_of_bu[bu] == i:
                    load_bu(bu + 2)
            for item in deferred:
                item[1]()
            for ui, (h, t) in enumerate(units):
                pb = ui % 2
                act(E2b[0][pb], mx(2 + h, t), AF.Square, [("mix", 2 + h, t)], [("E", 0, pb)])
                pe_mm(ps[pb][:], ones, E2b[0][pb], True, True, [("E", 0, pb)], [("ps", pb)])
                r_ = t1 if pb == 0 else t2
                rk = ("t1",) if pb == 0 else ("t2",)
                act(r_, ps[pb][:], AF.Sqrt, [("ps", pb)], [rk], scale=1.0 / P, bias=EPS)
                dve(lambda e, r_=r_: e.reciprocal(r_, r_), [rk], [rk])
                dve(lambda e, h=h, t=t, r_=r_, layer=layer: e.scalar_tensor_tensor(
                    out=mx(2 + h, t), in0=mx(2 + h, t), scalar=misc[:, 8 + layer:9 + layer], in1=r_, op0=ALU.mult, op1=ALU.mult),
                    [("mix", 2 + h, t), rk, ("misc",)], [("mix", 2 + h, t)])

        def fourier(scr, nblk, tab):
            kscr, vscr, gscr = scr
            pg.barrier()
            gctr = [0]
            gres = {}
            for kt in range(NT):
                first = True
                for b in range(nblk):
                    if gres.get(b) is None or nblk > 1:
                        s = gctr[0] % 2
                        gctr[0] += 1
                        dma("sp", Gslot[s], gscr[b], [("gscr", b)], [("G", s)])
                        gres = {b: s}
                    gs = gres[b]
                    for jg in range(4):
                        sl = load_slab_t(tab[kt, b, jg])
                        for jj in range(4):
                            j = jg * 4 + jj
                            last = (b == nblk - 1 and j == 15)
                            for cc in range(2):
                                for cs in range(2):
                                    pe_mm(ps[cc][:], Gslot[gs][:, j * 512 + cc * 256 + cs * P: j * 512 + cc * 256 + (cs + 1) * P],
                                          rslot(sl)[:, jj * 1024 + cs * 512: jj * 1024 + (cs + 1) * 512],
                                          first and cs == 0, last and cs == 1, [("G", gs), ("ring", sl)], [("ps", cc)])
                            first = False
                for cc in range(2):
                    act(mx(cc, kt), ps[cc][:], AF.Copy, [("ps", cc)], [("mix", cc, kt)])

        def outproj(layer):
            rot = [0]
            for s in range(2):
                sl = load_slab_w(w_out[layer], 0, KD, s * 512, 512)
                for ci in range(4):
                    dc = 4 * s + ci
                    for t in range(NT):
                        bank = 4 + rot[0] % 3
                        rot[0] += 1
                        for kc in range(KD):
                            pe_mm(ps[bank][:], rslot(sl)[:, kc * 512 + ci * P: kc * 512 + (ci + 1) * P], mx(kc, t),
                                  kc == 0, kc == KD - 1, [("ring", sl), ("mix", kc, t)], [("ps", bank)])
                        dve(lambda e, dc=dc, t=t, bank=bank: e.tensor_tensor(out=x_(dc, t), in0=ps[bank][:], in1=x_(dc, t),
                                                                             op=ALU.add),
                            [("ps", bank), ("x", dc, t)], [("x", dc, t)])

        ffctr = {"g": 0, "y": 0, "grp": 0}

        def ffn_groups(Wgu, Wd, F, expert=None):
            groups = []
            f0 = 0
            while f0 < F:
                fw = min(512, F - f0)
                groups.append((f0, fw))
                f0 += fw
            for (f0, fw) in groups:
                nci = fw // P
                gpar = ffctr["grp"] % 2
                ffctr["grp"] += 1
                sg_ = load_slab_w(Wgu, 0, KD, f0, fw)
                su_ = load_slab_w(Wgu, 0, KD, F + f0, fw)
                sd_ = load_slab_w(Wd, f0, nci, 0, D)

                def gu(t):
                    for ci in range(nci):
                        par = ffctr["g"] % 2
                        ffctr["g"] += 1
                        Gb, Ub = par, 2 + par
                        for kc in range(KD):
                            pe_mm(ps[Gb][:], rslot(sg_)[:, kc * fw + ci * P: kc * fw + (ci + 1) * P], mx(kc, t),
                                  kc == 0, kc == KD - 1, [("ring", sg_), ("mix", kc, t)], [("ps", Gb)])
                        for kc in range(KD):
                            pe_mm(ps[Ub][:], rslot(su_)[:, kc * fw + ci * P: kc * fw + (ci + 1) * P], mx(kc, t),
                                  kc == 0, kc == KD - 1, [("ring", su_), ("mix", kc, t)], [("ps", Ub)])
                        act(sg[par], ps[Gb][:], AF.Silu, [("ps", Gb)], [("sg", par)])
                        if expert is None:
                            dve(lambda e, par=par, Ub=Ub, ci=ci, t=t, gpar=gpar: e.tensor_tensor(
                                out=aT(gpar, ci, t), in0=ps[Ub][:], in1=sg[par], op=ALU.mult),
                                [("ps", Ub), ("sg", par)], [("aT", gpar, ci, t)])
                        else:
                            dve(lambda e, par=par, Ub=Ub: e.tensor_tensor(out=tmpf[par], in0=ps[Ub][:], in1=sg[par], op=ALU.mult),
                                [("ps", Ub), ("sg", par)], [("tmpf", par)])
                            dve(lambda e, par=par, ci=ci, t=t, gpar=gpar, expert=expert: e.tensor_tensor(
                                out=aT(gpar, ci, t), in0=tmpf[par], in1=bc_(expert % 2, t), op=ALU.mult),
                                [("tmpf", par), ("bc", expert % 2, t)], [("aT", gpar, ci, t)])

                def down(t):
                    for dc in range(KD):
                        yb = 4 + ffctr["y"] % 3
                        ffctr["y"] += 1
                        for ci in range(nci):
                            pe_mm(ps[yb][:], rslot(sd_)[:, ci * D + dc * P: ci * D + (dc + 1) * P], aT(gpar, ci, t),
                                  ci == 0, ci == nci - 1, [("ring", sd_), ("aT", gpar, ci, t)], [("ps", yb)])
                        dve(lambda e, dc=dc, t=t, yb=yb: e.tensor_tensor(out=x_(dc, t), in0=ps[yb][:], in1=x_(dc, t), op=ALU.add),
                            [("ps", yb), ("x", dc, t)], [("x", dc, t)])

                for step in range(NT + 1):
                    if step < NT:
                        gu(step)
                    if step > 0:
                        down(step - 1)

        def ffn_dense():
            pg.barrier()
            for t in range(NT):
                norm_tile(t, 16 + 0)
            ffn_groups(w_gu, w_d, FF_D)

        def ffn_moe():
            pg.barrier()
            for t in range(NT):
                norm_tile(t, 16 + 8, moe=True)
            for ex in range(NE):
                for t in range(NT):
                    pe_mm(ps[7][:], sel[:, ex * P:(ex + 1) * P], combT[:, t * TW:(t + 1) * TW], True, True,
                          [("combT", 4 * t + s_) for s_ in range(4)], [("ps", 7)])
                    act(bc_(ex % 2, t), ps[7][:], AF.Copy, [("ps", 7)], [("bc", ex % 2, t)])
                ffn_groups(e_gu[ex], e_d[ex], FF_E, expert=ex)

        def load_x(src):
            for t in range(NT):
                dma("sp", xT[:, :].rearrange("p (k s) -> p k s", k=KD)[:, :, t * TW:(t + 1) * TW],
                    src.rearrange("(k p) s -> p k s", p=P)[:, :, t * TW:(t + 1) * TW],
                    [], [("x", kc, t) for kc in range(KD)])

        def final_store(dst):
            pg.barrier()
            for t in range(NT):
                for kc in range(KD):
                    s_ = sq[kc % 2]
                    act(s_, x_(kc, t), AF.Square, [("x", kc, t)], [("sq", kc % 2)])
                    pe_mm(ps[7][:], ones, s_, kc == 0, kc == KD - 1, [("sq", kc % 2)], [("ps", 7)])
                r_ = rstd[t % 2]
                act(r_, ps[7][:], AF.Sqrt, [("ps", 7)], [("rstd", t % 2)], scale=1.0 / D, bias=EPS)
                dve(lambda e, r_=r_: e.reciprocal(r_, r_), [("rstd", t % 2)], [("rstd", t % 2)])
                for kc in range(KD):
                    dve(lambda e, kc=kc, t=t, r_=r_: e.scalar_tensor_tensor(
                        out=x_(kc, t), in0=x_(kc, t), scalar=gn[:, 32 + kc:33 + kc], in1=r_, op0=ALU.mult, op1=ALU.mult),
                        [("x", kc, t), ("rstd", t % 2)], [("x", kc, t)])
                dma("sp", dst.rearrange("(k p) s -> p k s", p=P)[:, :, t * TW:(t + 1) * TW],
                    xT[:, :].rearrange("p (k s) -> p k s", k=KD)[:, :, t * TW:(t + 1) * TW],
                    [("x", kc, t) for kc in range(KD)], [("yout",)])

        def dump(dst, src_t, nk, keys):
            pg.barrier()
            dma("sp", dst.rearrange("(k p) s -> p k s", p=P), src_t.rearrange("p (k s) -> p k s", k=nk), keys, [("dbg",)])
            pg.barrier()

        def dump_d2d(dst, src, keys):
            pg.barrier()
            dma("sp", dst, src, keys, [("dbg",)])
            pg.barrier()

        dma("sp", cst[:], cst_d[:, :], [], [("cst",)])
        dma("sp", ident[:], ident_d[:, :], [], [("ident",)])
        dma("sp", sel[:], sel_d[:, :], [], [("sel",)])
        dma("sp", gn[:], gn_d[:, :], [], [("gn",)])
        dma("sp", subg[:], sub_d[:, :], [], [("subg",)])
        dma("sp", lqk[:], lqk_d[:, :], [], [("lqk",)])
        dma("sp", wr_s[:], w_r[:, :], [], [("wr_s",)])
        dve(lambda e: e.memset(onesf[:], 1.0), [], [("onesf",)])
        MK = ("misc",)
        for l in range(2):
            for i in range(2):
                a0 = l * 256 + (2 * i) * 64
                dve(lambda e, a0=a0: e.tensor_tensor(out=sm[:, 0:64], in0=lqk[:, a0:a0 + 64], in1=lqk[:, a0 + 64:a0 + 128],
                                                     op=ALU.mult), [("lqk",)], [("sm",)])
                dve(lambda e, i=i: e.reduce_sum(misc[:, i:i + 1], sm[:, 0:64], axis=AX.X), [("sm",)], [MK])
                act(misc[:, 2 + i:3 + i], misc[:, i:i + 1], AF.Exp, [MK], [MK])
            dve(lambda e: e.tensor_tensor(out=misc[:, 12:13], in0=misc[:, 3:4], in1=misc[:, 2:3], op=ALU.subtract), [MK], [MK])
            dve(lambda e, l=l: e.tensor_scalar(out=misc[:, 4 + l:5 + l], in0=misc[:, 12:13], scalar1=-LAM_INIT[l], scalar2=None,
                                               op0=ALU.add), [MK], [MK])
            dve(lambda e, l=l: e.tensor_scalar(out=misc[:, 8 + l:9 + l], in0=subg[:, l:l + 1], scalar1=1.0 - LAM_INIT[l],
                                               scalar2=None, op0=ALU.mult), [("subg",), MK], [MK])
        for kc in range(KD):
            dve(lambda e, kc=kc: e.tensor_scalar(out=gwr[:, kc * NE:(kc + 1) * NE], in0=wr_s[:, kc * NE:(kc + 1) * NE],
                                                 scalar1=gn[:, 24 + kc:25 + kc], scalar2=None, op0=ALU.mult),
                [("wr_s",), ("gn",)], [("gwr",)])
        pg.barrier()

        scrP = (kP, vP, gP)
        for s in range(n_prompt):
            load_x(xp[s])
            for layer in range(2):
                dbg = debug and s == 0
                if dbg:
                    pg.barrier()
                    for t in range(NT):
                        norm_tile(t, layer * 8)
                    dump(dbg_h[layer], mix[:, :], KD, [])
                inproj(layer, ropeP, scrP, 0)
                if dbg:
                    dump(dbg_q[layer], R[:, 0:12288], NH, [])
                    dump_d2d(dbg_k[layer], kP[0], [("kscr", 0)])
                    dump_d2d(dbg_v[layer], vP[0], [("vscr", 0)])
                    dump_d2d(dbg_g[layer], gP[0], [("gscr", 0)])
                attention(layer, scrP, 1)
                fourier(scrP, 1, dftP)
                if dbg:
                    dump(dbg_mix[layer], mix[:, :], KD, [])
                outproj(layer)
                if dbg:
                    dump(dbg_xm[layer], xT[:, :], KD, [])
                if layer == 0:
                    ffn_dense()
                else:
                    ffn_moe()
                if dbg:
                    dump(dbg_xf[layer], xT[:, :], KD, [])
            final_store(yp[s])
        if do_sample:
            scr0 = (kS[0], vS[0], gS[0])
            scr1 = (kS[1], vS[1], gS[1])
            for j in range(4):
                load_x(xs[j])
                inproj(0, ropeS[j], scr0, j, do_q=False, do_kvg=True)
            for j in range(4):
                load_x(xs[j])
                inproj(0, ropeS[j], scr0, j, do_q=True, do_kvg=False)
                attention(0, scr0, 4)
                fourier(scr0, 4, dftS[j])
                outproj(0)
                ffn_dense()
                inproj(1, ropeS[j], scr1, j, do_q=(j == 3), do_kvg=True)
            attention(1, scr1, 4)
            fourier(scr1, 4, dftS[3])
            outproj(1)
            ffn_moe()
            final_store(ys)
        pg.barrier()

        pg.finalize()
        sems = {}

        def get_sem(key):
            if key not in sems:
                sems[key] = es.enter_context(nc.semaphore("s_" + "_".join(str(k) for k in key)))
            return sems[key]

        for e in Prog.ENGS:
            for ep in range(pg.n_epochs[e]):
                get_sem((e, "c", ep))
        for q in ("pool", "sp"):
            for i in range(NDS):
                get_sem((q, i))

        by_eng = {e: [] for e in Prog.ENGS}
        for o in pg.ops:
            by_eng[o.eng].append(o)
        ops = pg.ops

        def emit(eng_name, handle):
            waited = {}
            lst = by_eng[eng_name]
            for o in lst:
                need = {}
                for d in o.deps:
                    p = ops[d]
                    if p.sem[1] == "c":
                        key = p.sem
                    else:
                        key = p.sem
                    if need.get(key, 0) < p.semval:
                        need[key] = p.semval
                for key, val in need.items():
                    if waited.get(key, 0) >= val:
                        continue
                    waited[key] = val
                    handle.wait_ge(get_sem(key), val)
                ins = o.fn(handle)
                if o.marked:
                    ins.then_inc(get_sem(o.sem), 16 if o.dma else 1)
            if eng_name in ("sp", "pool"):
                lastv = {}
                for o in lst:
                    if o.dma:
                        lastv[o.sem] = max(lastv.get(o.sem, 0), o.semval)
                for key, val in lastv.items():
                    if waited.get(key, 0) < val:
                        handle.wait_ge(get_sem(key), val)

        with nc.Block() as block:
            @block.tensor
            def _(t):
                emit("pe", t)

            @block.scalar
            def _(s):
                emit("act", s)

            @block.vector
            def _(v):
                emit("dve", v)

            @block.gpsimd
            def _(g):
                emit("pool", g)

            @block.sync
            def _(sy):
                emit("sp", sy)
    return nc, len(pg.ops)


def _rope_tab(pos0):
    inv = (np.float32(500000.0) ** (-(np.arange(0, 16, 2, dtype=np.float32) / np.float32(16)))).astype(np.float32)
    pos = np.arange(pos0, pos0 + SEG).astype(np.float32)
    ang = (pos[:, None] * inv[None, :]).astype(np.float32).astype(np.float64)
    cos = np.ones((P, SEG), np.float32)
    sin = np.zeros((P, SEG), np.float32)
    for base in (0, 64):
        for i in range(8):
            cos[base + i] = np.cos(ang[:, i])
            cos[base + 8 + i] = np.cos(ang[:, i])
            sin[base + i] = -np.sin(ang[:, i])
            sin[base + 8 + i] = np.sin(ang[:, i])
    return np.stack([cos, sin], 0)


def _dft_block(S, s0, k0):
    s = (s0 + np.arange(SEG)).astype(np.int64)
    k = (k0 + np.arange(SEG)).astype(np.int64)
    ph = (s[:, None] * k[None, :]) % S
    ang = ph.astype(np.float64) * (2.0 * np.pi / S)
    scale = 1.0 / math.sqrt(S * 64.0)
    c = (np.cos(ang) * scale).astype(np.float32)
    sn = (-np.sin(ang) * scale).astype(np.float32)
    t = np.stack([c, sn], 1)
    t = t.reshape(4, 4, P, 2, 4, TW)
    t = t.transpose(4, 0, 2, 1, 3, 5)
    return np.ascontiguousarray(t).reshape(4, 4, P, SLAB).astype(ml_dtypes.bfloat16)


def _consts():
    cst = np.zeros((P, 512), np.float32)
    cst[:, 0:128] = 1.0
    for base in (0, 64):
        for m in range(8):
            cst[base + m + 8, 128 + base + m] = 1.0
            cst[base + m, 128 + base + m + 8] = 1.0
    c = np.arange(64)
    ang = 2.0 * np.pi * ((c[:, None] * c[None, :]) % 64) / 64.0
    for g in range(2):
        cst[g * 64:(g + 1) * 64, 256 + g * 64: 256 + (g + 1) * 64] = np.cos(ang)
        cst[g * 64:(g + 1) * 64, 384 + g * 64: 384 + (g + 1) * 64] = np.sin(ang)
    ident = np.eye(P, dtype=np.float32)
    sel = np.zeros((NE, NE * P), np.float32)
    for e in range(NE):
        sel[e, e * P:(e + 1) * P] = 1.0
    return cst.astype(ml_dtypes.bfloat16), ident, sel.astype(ml_dtypes.bfloat16)


_CACHE = {}


def kernel(x_prompt, x_sample, norm_mix, w_in, lambda_qk, subln_gain, w_out, norm_ffn,
           ffn_w_gate_up, ffn_w_down, router_w, expert_w_gate_up, expert_w_down, final_norm,
           _n_prompt=4, _do_sample=True, _debug=False):
    f32 = np.float32
    x_prompt = np.asarray(x_prompt, f32); x_sample = np.asarray(x_sample, f32)
    n_cores = 8
    key = (_n_prompt, _do_sample, _debug)
    if key not in _CACHE:
        _CACHE[key] = build_program(_n_prompt, _do_sample, _debug)
    nc, _ = _CACHE[key]

    cst, ident, sel = _consts()
    gn = np.zeros((P, 40), f32)
    nm = np.asarray(norm_mix, f32); nf = np.asarray(norm_ffn, f32); fn_ = np.asarray(final_norm, f32)
    for l in range(2):
        gn[:, l * 8:(l + 1) * 8] = nm[l].reshape(KD, P).T
        gn[:, 16 + l * 8:16 + (l + 1) * 8] = nf[l].reshape(KD, P).T
    gn[:, 32:40] = fn_.reshape(KD, P).T
    sub = np.ascontiguousarray(np.asarray(subln_gain, f32).T)
    lqk = np.ascontiguousarray(np.broadcast_to(np.asarray(lambda_qk, f32).reshape(1, 512), (P, 512)))
    wr = np.ascontiguousarray(np.asarray(router_w, f32)[0].reshape(KD, P, NE).transpose(1, 0, 2).reshape(P, KD * NE))
    ropeP = _rope_tab(0)
    dftP = _dft_block(2048, 0, 0).reshape(4, 1, 4, P, SLAB)
    dft_blocks = {}
    if _do_sample:
        for a in range(4):
            for b in range(4):
                dft_blocks[(a, b)] = _dft_block(8192, b * SEG, a * SEG)
    common = dict(
        w_in=np.asarray(w_in, f32), w_out=np.asarray(w_out, f32),
        w_gu=np.asarray(ffn_w_gate_up, f32)[0], w_d=np.asarray(ffn_w_down, f32)[0], w_r=wr,
        e_gu=np.asarray(expert_w_gate_up, f32)[0], e_d=np.asarray(expert_w_down, f32)[0],
        gn=gn, sub=sub, lqk=lqk, ropeP=ropeP, dftP=dftP, cst=cst, ident=ident, sel=sel,
    )
    npc = 4
    in_maps = []
    orders = []
    for c in range(n_cores):
        m = dict(common)
        if _n_prompt > 0:
            xpc = x_prompt[c * npc: c * npc + _n_prompt]
            m["xp"] = np.ascontiguousarray(xpc.transpose(0, 2, 1))
        else:
            m["xp"] = np.zeros((1, D, SEG), f32)
        sq_, own = c // 4, c % 4
        order = [q for q in range(4) if q != own] + [own]
        orders.append(order)
        if _do_sample:
            xsq = x_sample[sq_].reshape(4, SEG, D)
            m["xs"] = np.ascontiguousarray(np.stack([xsq[q].T for q in order], 0))
            m["ropeS"] = np.stack([_rope_tab(q * SEG) for q in order], 0)
            m["dftS"] = np.stack([np.stack([dft_blocks[(a, b)] for b in order], 1) for a in order], 0)
        else:
            m["xs"] = np.zeros((4, D, SEG), f32)
            m["ropeS"] = np.zeros((4, 2, P, SEG), f32)
            m["dftS"] = np.zeros((4, 4, 4, 4, P, SLAB), ml_dtypes.bfloat16)
        in_maps.append(m)
    res = run_bass_kernel_spmd(nc, in_maps, core_ids=list(range(n_cores)))
    if _debug:
        _CACHE["dbg"] = {k: np.asarray(v) for k, v in res.results[0].items() if k.startswith("dbg")}
    y_prompt = np.zeros((32, SEG, D), f32)
    y_sample = np.zeros((2, 8192, D), f32)
    for c in range(n_cores):
        r = res.results[c]
        if _n_prompt > 0:
            y_prompt[c * npc: c * npc + _n_prompt] = np.asarray(r["yp"]).transpose(0, 2, 1)
        if _do_sample:
            sq_, own = c // 4, c % 4
            y_sample[sq_, own * SEG:(own + 1) * SEG] = np.asarray(r["ys"]).T
    return (y_prompt, y_sample)
```
